# Optimizing a Trainium2 kernel written in Bass

```python
import jax, jax.numpy as jnp
from jax import lax
import numpy as np

D_MODEL = 1024
BATCH = 16
SEQ = 4096
DEPTH = 1

GLA_HEADS = 4
GLA_WIDTH = 512
GLA_DV = GLA_WIDTH // GLA_HEADS
GLA_DK = GLA_DV // 2
GLA_KEY = GLA_HEADS * GLA_DK
GATE_RANK = 16
GATE_NORMALIZER = 16.0
GLA_CHUNK = 64

SG_GROUPS = 4
SG_WIDTH = 512
SG_CH = SG_WIDTH // SG_GROUPS
SG_CHUNK = 128

MIX_WIDTH = GLA_WIDTH + SG_WIDTH

D_FF = -(-8 * D_MODEL // (3 * 256)) * 256

NORM_EPS = 1e-5

W_Q = GLA_KEY
W_K = GLA_KEY
W_V = GLA_WIDTH
W_GLR = GATE_RANK
W_GOUT = GLA_WIDTH
W_SU = SG_WIDTH
W_SV = SG_WIDTH
IN_COLS = W_Q + W_K + W_V + W_GLR + W_GOUT + W_SU + W_SV
SPLITS = (W_Q,
          W_Q + W_K,
          W_Q + W_K + W_V,
          W_Q + W_K + W_V + W_GLR,
          W_Q + W_K + W_V + W_GLR + W_GOUT,
          W_Q + W_K + W_V + W_GLR + W_GOUT + W_SU)

kernel_name = "gla_sgu_parallel_hybrid_block"


def rms_norm(x, w):
    xf = x.astype(jnp.float32)
    y = xf * lax.rsqrt(jnp.mean(xf * xf, axis=-1, keepdims=True) + NORM_EPS)
    return (y * w.astype(jnp.float32)).astype(x.dtype)


def layer_norm(x, w, b):
    xf = x.astype(jnp.float32)
    mu = jnp.mean(xf, axis=-1, keepdims=True)
    var = jnp.mean(jnp.square(xf - mu), axis=-1, keepdims=True)
    y = (xf - mu) * lax.rsqrt(var + NORM_EPS)
    return (y * w.astype(jnp.float32) + b.astype(jnp.float32)).astype(x.dtype)


def gla_chunked(q, k, v, log_a):
    B, T, H, DK = q.shape
    DV = v.shape[-1]
    C = GLA_CHUNK
    N = T // C

    def blk(t):
        return t.reshape(B, N, C, H, t.shape[-1])

    q, k, v, log_a = blk(q), blk(k), blk(v), blk(log_a)
    b = jnp.cumsum(log_a, axis=2)
    b_last = b[:, :, -1:]
    q_dec = q * jnp.exp(b)
    k_inv = k * jnp.exp(-b)
    k_end = k * jnp.exp(b_last - b)

    scores = jnp.einsum('bnihd,bnjhd->bnhij', q_dec, k_inv)
    causal = jnp.tril(jnp.ones((C, C), dtype=bool))
    scores = jnp.where(causal, scores, jnp.zeros((), scores.dtype))
    o_intra = jnp.einsum('bnhij,bnjhe->bnihe', scores, v)

    u = jnp.einsum('bnjhd,bnjhe->nbhde', k_end, v)
    decay = jnp.moveaxis(jnp.exp(b_last[:, :, 0]), 1, 0)

    def step(state, inp):
        d, un = inp
        return d[..., None] * state + un, state

    s0 = jnp.zeros((B, H, DK, DV), q.dtype)
    _, s_prev = lax.scan(step, s0, (decay, u))
    o_inter = jnp.einsum('bnihd,nbhde->bnihe', q_dec, s_prev)
    return (o_intra + o_inter).reshape(B, T, H, DV)


def spatial_gate(u, v, ln_w, ln_b, w_s, b_s):
    B, T, G, Cc = v.shape
    N = T // SG_CHUNK
    v = layer_norm(v, ln_w, ln_b).reshape(B, N, SG_CHUNK, G, Cc)
    causal = jnp.tril(jnp.ones((SG_CHUNK, SG_CHUNK), dtype=bool))
    w = jnp.where(causal, w_s, jnp.zeros((), w_s.dtype))
    mixed = jnp.einsum('gts,bnsgc->bntgc', w, v) + jnp.transpose(b_s)[None, None, :, :, None]
    return u * mixed.reshape(B, T, G, Cc)


def setup_inputs(seed: int = 0) -> dict:
    key = jax.random.key(seed)
    ks = jax.random.split(key, 20)
    f32 = jnp.float32

    def nrm(k, shape, scale):
        return jax.random.normal(k, shape, f32) * scale

    return {
        "x": jax.random.normal(ks[0], (BATCH, SEQ, D_MODEL), f32),
        "norm1_w": 1.0 + nrm(ks[1], (DEPTH, D_MODEL), 0.02),
        "w_in": nrm(ks[2], (DEPTH, D_MODEL, IN_COLS), D_MODEL ** -0.5),
        "w_gate_up": nrm(ks[3], (DEPTH, GATE_RANK, GLA_KEY), GATE_RANK ** -0.5),
        "b_gate_up": nrm(ks[4], (DEPTH, GLA_KEY), 0.1),
        "gla_norm_w": 1.0 + nrm(ks[5], (DEPTH, GLA_DV), 0.02),
        "sg_ln_w": 1.0 + nrm(ks[6], (DEPTH, SG_GROUPS, SG_CH), 0.02),
        "sg_ln_b": nrm(ks[7], (DEPTH, SG_GROUPS, SG_CH), 0.02),
        "sg_w_s": nrm(ks[8], (DEPTH, SG_GROUPS, SG_CHUNK, SG_CHUNK), SG_CHUNK ** -0.5),
        "sg_b_s": 1.0 + nrm(ks[9], (DEPTH, SG_GROUPS, SG_CHUNK), 0.02),
        "w_out": nrm(ks[10], (DEPTH, MIX_WIDTH, D_MODEL), MIX_WIDTH ** -0.5),
        "norm2_w": 1.0 + nrm(ks[11], (DEPTH, D_MODEL), 0.02),
        "w_ffn_gate": nrm(ks[12], (DEPTH, D_MODEL, D_FF), D_MODEL ** -0.5),
        "w_ffn_up": nrm(ks[13], (DEPTH, D_MODEL, D_FF), D_MODEL ** -0.5),
        "w_ffn_down": nrm(ks[14], (DEPTH, D_FF, D_MODEL), D_FF ** -0.5),
        "final_norm_w": 1.0 + nrm(ks[15], (D_MODEL,), 0.02),
    }


def reference(x, norm1_w, w_in, w_gate_up, b_gate_up, gla_norm_w, sg_ln_w, sg_ln_b,
              sg_w_s, sg_b_s, w_out, norm2_w, w_ffn_gate, w_ffn_up, w_ffn_down,
              final_norm_w):
    B, T, _ = x.shape
    f32 = jnp.float32
    h = x
    for l in range(DEPTH):
        n = rms_norm(h, norm1_w[l])
        z = jnp.einsum('btd,dc->btc', n, w_in[l])
        q, k, v, g_lr, g_out, su, sv = jnp.split(z, SPLITS, axis=-1)

        qh = q.reshape(B, T, GLA_HEADS, GLA_DK).astype(f32) * (GLA_DK ** -0.5)
        kh = k.reshape(B, T, GLA_HEADS, GLA_DK).astype(f32)
        vh = v.reshape(B, T, GLA_HEADS, GLA_DV).astype(f32)
        gk = jnp.einsum('btr,rk->btk', g_lr, w_gate_up[l]) + b_gate_up[l]
        log_a = (jax.nn.log_sigmoid(gk.astype(f32)) / GATE_NORMALIZER).reshape(B, T, GLA_HEADS, GLA_DK)
        o = gla_chunked(qh, kh, vh, log_a)
        o = rms_norm(o, gla_norm_w[l]) * jax.nn.silu(g_out.reshape(B, T, GLA_HEADS, GLA_DV).astype(f32))
        o_gla = o.reshape(B, T, GLA_WIDTH).astype(x.dtype)

        su = jax.nn.gelu(su, approximate=False).reshape(B, T, SG_GROUPS, SG_CH)
        sv = jax.nn.gelu(sv, approximate=False).reshape(B, T, SG_GROUPS, SG_CH)
        o_sg = spatial_gate(su, sv, sg_ln_w[l], sg_ln_b[l], sg_w_s[l], sg_b_s[l]).reshape(B, T, SG_WIDTH)

        mix = jnp.concatenate([o_gla, o_sg], axis=-1)
        h = h + jnp.einsum('btc,cd->btd', mix, w_out[l])

        n2 = rms_norm(h, norm2_w[l])
        a = jnp.einsum('btd,df->btf', n2, w_ffn_gate[l])
        bu = jnp.einsum('btd,df->btf', n2, w_ffn_up[l])
        h = h + jnp.einsum('btf,fd->btd', jax.nn.silu(a) * bu, w_ffn_down[l])
    return rms_norm(h, final_norm_w)
```

```python
import numpy as np
import ml_dtypes
import concourse.bass as bass
import concourse.mybir as mybir
from concourse.bass_utils import run_bass_kernel_spmd

F32 = mybir.dt.float32
BF16 = mybir.dt.bfloat16
AF = mybir.ActivationFunctionType
ALU = mybir.AluOpType
AX = mybir.AxisListType

P = 128
D = 1024
KD = 8
TB = 512
NT = 4
DFF = 2816
NF = 22
NCORES = 8
EPS = 1e-5
INC = 2576
NR = 12
import os as _os
LATSCALE = float(_os.environ.get('LATSCALE', '1.0'))

SC_WIN = 0
SC_WOUT = 9
SC_GU = 17
SC_D = 61
SC_N = 83
FM_COLS = [(0, 128), (128, 128), (256, 128), (384, 128), (1024, 16),
           (1552, 128), (1680, 128), (1808, 128), (1936, 128)]


class Sched:
    def __init__(self):
        self.ops = []
        self.last_w = {}
        self.readers = {}
        self.dma_cnt = {}

    def add(self, eng, fn, r=(), w=(), dma=None):
        i = len(self.ops)
        deps = set()
        for k in r:
            if k in self.last_w:
                deps.add(self.last_w[k])
        for k in w:
            if k in self.last_w:
                deps.add(self.last_w[k])
            deps.update(self.readers.get(k, ()))
        for k in r:
            if fn is not None:
                self.readers.setdefault(k, []).append(i)
        for k in w:
            self.last_w[k] = i
            self.readers[k] = []
        deps.discard(i)
        op = dict(eng=eng, fn=fn, r=frozenset(r), w=frozenset(w), deps=deps, dma=dma,
                  sig=None, need=False)
        if dma is not None:
            n = self.dma_cnt.get(dma, 0) + 1
            self.dma_cnt[dma] = n
            op['sig'] = (dma, 16 * n)
        self.ops.append(op)
        return i

    def finalize(self):
        ops = self.ops
        for op in ops:
            keep = []
            for d in op['deps']:
                p = ops[d]
                if p['dma'] is None and op['dma'] is None and p['eng'] == op['eng']:
                    if op['eng'] == 'pe':
                        continue
                    if not (p['w'] & op['r']):
                        continue
                keep.append(d)
                p['need'] = True
            op['deps'] = keep
        cnt = {}
        for op in ops:
            if op['dma'] is None and op['need']:
                c = cnt.get(op['eng'], 0) + 1
                cnt[op['eng']] = c
                op['sig'] = (op['eng'], c)

    def emit(self, nc):
        self.finalize()
        ops = self.ops
        keys = []
        for op in ops:
            if op['sig'] is not None and op['sig'][0] not in keys:
                keys.append(op['sig'][0])
        sems = {k: nc.alloc_semaphore("s_%d" % i) for i, k in enumerate(keys)}
        engmap = {'pe': 'tensor', 'act': 'scalar', 'dve': 'vector', 'pool': 'gpsimd', 'sp': 'sync'}
        with nc.Block() as block:
            for eng, attr in engmap.items():
                mine = [op for op in ops if op['eng'] == eng]

                def body(e, mine=mine):
                    seen = {}
                    for op in mine:
                        need = {}
                        for d in op['deps']:
                            s, v = ops[d]['sig']
                            if seen.get(s, 0) < v and need.get(s, 0) < v:
                                need[s] = v
                        for s, v in need.items():
                            e.wait_ge(sems[s], v)
                            seen[s] = v
                        if op['fn'] is None:
                            continue
                        ins = op['fn'](e)
                        if op['sig'] is not None:
                            ins.then_inc(sems[op['sig'][0]], 16 if op['dma'] is not None else 1)

                getattr(block, attr)(body)


def build(n_tok, seq_len, stop=None):
    nblk = n_tok // TB
    bps = seq_len // TB
    nc = bass.Bass("TRN2", target_bir_lowering=False)
    S = Sched()

    def din(name, shape, dt=F32):
        return nc.dram_tensor(name, list(shape), dt, kind="ExternalInput").ap()

    x = din("x", [n_tok, D])
    out = nc.dram_tensor("out", [n_tok, D], F32, kind="ExternalOutput").ap()
    w_in = din("w_in", [D, INC])
    w_out = din("w_out", [D, D])
    w_g = din("w_g", [D, DFF])
    w_u = din("w_u", [D, DFF])
    w_d = din("w_d", [DFF, D])
    n1b = din("n1b", [P, D])
    n2b = din("n2b", [P, D])
    nfb = din("nfb", [P, D])
    wgu = din("wgu", [17, 256])
    glaw = din("glaw", [P, 1])
    lnw = din("lnw", [P, 4])
    lnb = din("lnb", [P, 4])
    wsT = din("wsT", [P, 512])
    bsb = din("bsb", [P, 512])
    c_ident = din("c_ident", [P, P], BF16)
    c_u4 = din("c_u4", [P, 512], BF16)
    c_su = din("c_su", [P, P], BF16)
    wscr = nc.dram_tensor("wscr", [SC_N, P, 1024], BF16, kind="Internal").ap()

    def sb(name, shape, dt):
        return nc.alloc_sbuf_tensor(name, list(shape), dt)

    xh = [sb("xh%d" % i, [P, D], F32) for i in range(8)]
    w1b = sb("w1b", [P, D], F32)
    w2b = sb("w2b", [P, D], F32)
    wfb = sb("wfb", [P, D], F32)
    WINT = sb("WINT", [P, KD, 1792], BF16)
    ring = sb("ring", [P, NR, 1024], BF16)
    nbf = sb("nbf", [P, D], BF16)
    nbf2 = sb("nbf2", [P, D], BF16)
    junk = sb("junk", [P, D], BF16)
    nT = sb("nT", [P, KD, TB], BF16)
    n2T = sb("n2T", [P, KD, TB], BF16)
    qT = sb("qT", [P, 2, TB], F32)
    kT = sb("kT", [P, 2, TB], F32)
    glrT = sb("glrT", [32, TB], BF16)
    v_tm = sb("v_tm", [P, NT, 512], BF16)
    k_tm = sb("k_tm", [P, NT, 256], F32)
    sg_tm = sb("sg_tm", [P, NT, 512], BF16)
    suT = sb("suT", [P, 4, TB], BF16)
    sv_tm = sb("sv_tm", [P, NT, 512], BF16)
    vhat = [sb("vhat%d" % i, [P, 512], BF16) for i in range(2)]
    e_t = sb("e_t", [P, 256], F32)
    l_t = sb("l_t", [P, 256], F32)
    lhi = sb("lhi", [P, 256], BF16)
    llo = sb("llo", [P, 256], BF16)
    exq = sb("exq", [P, 256], F32)
    exk = sb("exk", [P, 256], F32)
    eR = sb("eR", [P, 256], F32)
    dec = sb("dec", [P, 2], F32)
    qTdz = sb("qTdz", [P, 2, 2, P], BF16)
    kTi = sb("kTi", [P, 2, P], BF16)
    kend = sb("kend", [P, 256], BF16)
    scT = sb("scT", [P, 512], BF16)
    sq_t = sb("sq_t", [P, 512], F32)
    og = sb("og", [P, 512], BF16)
    Sst = sb("Sst", [P, 2, P], F32)
    Sbf = [sb("Sbf%d" % i, [P, 2, P], BF16) for i in range(2)]
    sgt = sb("sgt", [P, 512], F32)
    mixT = sb("mixT", [P, KD, TB], BF16)
    gT = sb("gT", [P, 11, TB], BF16)
    tmpF = [sb("tmpF%d" % i, [P, TB], F32) for i in range(2)]
    ident = sb("ident", [P, P], BF16)
    U4 = sb("U4", [P, 512], BF16)
    SU = sb("SU", [P, P], BF16)
    ones_bf = sb("ones_bf", [P, P], BF16)
    ones_f = sb("ones_f", [P, P], F32)
    WsTm = sb("WsTm", [P, 512], BF16)
    wsT_f = sb("wsT_f", [P, 512], F32)
    bsb_t = sb("bsb_t", [P, 512], F32)
    Rc = sb("Rc", [P, 512], F32)
    LW = sb("LW", [P, 512], F32)
    lnw_t = sb("lnw_t", [P, 4], F32)
    lnb_t = sb("lnb_t", [P, 4], F32)
    glaw_t = sb("glaw_t", [P, 1], F32)
    wgu_f = sb("wgu_f", [32, 256], F32)
    wgu_b = sb("wgu_b", [32, 256], BF16)
    eps_t = sb("eps_t", [P, 1], F32)
    one_t = sb("one_t", [P, 1], F32)
    st = sb("st", [P, 24], F32)
    st2 = sb("st2", [P, 24], F32)
    rs = sb("rs", [P, 24], F32)
    bst = sb("bst", [P, 4, 6], F32)
    mv = sb("mv", [P, 4, 2], F32)

    psb = [nc.alloc_psum_tensor("ps%d" % i, [P, 512], F32) for i in range(8)]
    psb16 = [t.bitcast(BF16) for t in psb]
    state = dict(bank=0, rpos=0)

    bstate = {'m': 0, 'f': 0}

    def bank(th='m'):
        b = bstate[th]
        bstate[th] = (b + 1) % 4
        return b + (0 if th == 'm' else 4)

    pe = lambda fn, r, w: S.add('pe', fn, r, w)
    act = lambda fn, r, w: S.add('act', fn, r, w)
    dve = lambda fn, r, w: S.add('dve', fn, r, w)
    pool_eng = ['dve']
    pool = lambda fn, r, w: S.add(pool_eng[0], fn, r, w)

    def mm(o, l, rh, st_, sp_, r, w):
        pe(lambda e: e.matmul(o, l, rh, start=st_, stop=sp_), r, w)

    def tr(o, i_, r, w):
        pe(lambda e: e.transpose(o, i_, ident[:, :]), r + ['ident'], w)

    npd = [0]

    def pdma(o, i_, r, w, key):
        S.add('pool', lambda e: e.dma_start(out=o, in_=i_), r, w, dma=key)
        if key[0] == 'g':
            npd[0] += 1
            if npd[0] % 16 == 0:
                S.add('pool', None, list(w), [])

    def sdma(o, i_, r, w, key):
        S.add('sp', lambda e: e.dma_start(out=o, in_=i_), r, w, dma=key)

    for (t_, d_, nm) in [(ident, c_ident, 'ident'), (U4, c_u4, 'U4'), (SU, c_su, 'SU'),
                         (w1b, n1b, 'w1b'), (w2b, n2b, 'w2b'), (wfb, nfb, 'wfb'),
                         (lnw_t, lnw, 'lnw'), (lnb_t, lnb, 'lnb'), (glaw_t, glaw, 'glaw'),
                         (wsT_f, wsT, 'wsT_f'), (bsb_t, bsb, 'bsb')]:
        sdma(t_[:], d_[:], [], [nm], ('c', nm))
    sdma(wgu_f[0:17, :], wgu[:, :], [], ['wgu_f'], ('c', 'wgu'))

    for t in range(NT):
        S.add('pool', (lambda e, t=t: e.dma_start(out=xh[t][:, :], in_=x[t * P:(t + 1) * P, :])), [], [('xh', t)], dma=('xs', t))
    if nblk > 1:
        for t in range(NT):
            S.add('pool', (lambda e, t=t: e.dma_start(out=xh[4 + t][:, :], in_=x[TB + t * P:TB + (t + 1) * P, :])),
                  [], [('xh', 4 + t)], dma=('xs', 4 + t))
    w_in_v = w_in.rearrange("(k p) c -> p k c", p=P)
    for (c0, wd, o0) in [(512, 512, 0), (256, 256, 512), (1040, 512, 768), (2064, 512, 1280)]:
        pdma(WINT[:, :, o0:o0 + wd], w_in_v[:, :, c0:c0 + wd], [], ['WINT'], ('g', 'wint'))
    for m, (c0, wd) in enumerate(FM_COLS):
        pdma(wscr[SC_WIN + m][:, 0:KD * wd].rearrange("p (k c) -> p k c", c=wd),
             w_in_v[:, :, c0:c0 + wd], [], ['scrA'], ('g', 'scrA'))
    pdma(wscr[SC_WOUT:SC_WOUT + 8], w_out.rearrange("(c p) d -> c p d", p=P), [], ['scrB'], ('g', 'scrB'))
    w_g_v = w_g.rearrange("(k p) f -> p k f", p=P)
    w_u_v = w_u.rearrange("(k p) f -> p k f", p=P)
    w_d_v = w_d.rearrange("(j p) d -> j p d", p=P)
    for j in range(NF):
        for gu, wv in enumerate((w_g_v, w_u_v)):
            pdma(wscr[SC_GU + 2 * j + gu].rearrange("p (k f) -> p k f", f=P),
                 wv[:, :, j * P:(j + 1) * P], [], ['scrC%d' % (j // 3)], ('g', 'scrC%d' % (j // 3)))
        if j % 6 == 5 or j == NF - 1:
            j0 = (j // 6) * 6
            pdma(wscr[SC_D + j0:SC_D + j + 1], w_d_v[j0:j + 1], [], ['scrD%d' % (j // 6)], ('g', 'scrD%d' % (j // 6)))

    dve(lambda e: e.memset(eps_t[:], EPS), [], ['eps'])
    dve(lambda e: e.memset(one_t[:], 1.0), [], ['one'])
    dve(lambda e: e.memset(ones_bf[:], 1.0), [], ['ones_bf'])
    dve(lambda e: e.memset(ones_f[:], 1.0), [], ['ones_f'])
    dve(lambda e: e.memset(glrT[:], 1.0), [], ['glrT'])
    dve(lambda e: e.memset(qTdz[:], 0.0), [], ['qTd'])
    dve(lambda e: e.tensor_copy(wgu_b[0:17, :], wgu_f[0:17, :]), ['wgu_f'], ['wgu_b'])
    dve(lambda e: e.tensor_tensor(out=WsTm[:], in0=wsT_f[:], in1=U4[:], op=ALU.mult),
        ['wsT_f', 'U4'], ['WsTm'])
    b0 = bank()
    mm(psb[b0][:, :], ones_bf[:, :], WsTm[:, :], True, True, ['ones_bf', 'WsTm'], [('ps', b0)])
    for g in range(4):
        gs = slice(g * P, (g + 1) * P)
        dve(lambda e, g=g, gs=gs: e.scalar_tensor_tensor(
            out=Rc[:, gs], in0=psb[b0][:, gs], scalar=lnb_t[:, g:g + 1], in1=bsb_t[:, gs],
            op0=ALU.mult, op1=ALU.add), [('ps', b0), 'lnb', 'bsb'], ['Rc'])
        dve(lambda e, g=g, gs=gs: e.tensor_scalar(
            out=LW[:, gs], in0=ones_f[:, :], scalar1=lnw_t[:, g:g + 1], scalar2=None, op0=ALU.mult),
            ['ones_f', 'lnw'], ['LW'])

    def ring_next(idx, key, width=1024):
        s = state['rpos'] % NR
        state['rpos'] += 1
        sdma(ring[:, s, 0:width], wscr[idx][:, 0:width], [key], [('ring', s)], ('ring', s))
        return s

    def load_x(b, tiles):
        for t in tiles:
            sl = (b % 2) * 4 + t
            r0 = b * TB + t * P
            pdma(xh[sl][:, :], x[r0:r0 + P, :], [], [('xh', sl)], ('xs', sl))

    def rstd_from(col, scale, n=1):
        cs = slice(col, col + n)
        act(lambda e: e.activation(out=st2[:, cs], in_=st[:, cs], func=AF.Ln, bias=eps_t[:, 0:1], scale=scale),
            [('st', col), 'eps'], [('st2', col)])
        act(lambda e: e.activation(out=rs[:, cs], in_=st2[:, cs], func=AF.Exp, scale=-0.5),
            [('st2', col)], [('rs', col)])

    def sigmoid_act(dst, src, dkey, skey):
        act(lambda e: e.activation(out=dst, in_=src, func=AF.Exp, scale=-1.0), [skey], [dkey])
        act(lambda e: e.activation(out=dst, in_=dst, func=AF.Ln, bias=one_t[:, 0:1], scale=1.0), [dkey, 'one'], [dkey])
        act(lambda e: e.activation(out=dst, in_=dst, func=AF.Exp, scale=-1.0), [dkey], [dkey])

    def norm_T(sl, t, col, wb, wbk, dstT, dkey, nb_, nbk, th):
        act(lambda e: e.activation(out=junk[:, :], in_=xh[sl][:, :], func=AF.Square,
                                   accum_out=st[:, col:col + 1]), [('xh', sl)], [('st', col)])
        rstd_from(col, 1.0 / D)
        dve(lambda e: e.scalar_tensor_tensor(out=nb_[:, :], in0=xh[sl][:, :], scalar=rs[:, col:col + 1],
                                             in1=wb[:, :], op0=ALU.mult, op1=ALU.mult),
            [('xh', sl), ('rs', col), wbk], [nbk])
        yield (0.0, 4.0)
        bk = bank(th)
        for k in range(KD):
            tr(psb16[bk][:, k * P:(k + 1) * P], nb_[:, k * P:(k + 1) * P], [nbk], [('ps', bk)])
        act(lambda e: e.activation(out=dstT[:, :, t * P:(t + 1) * P],
                                   in_=psb16[bk][:, :].rearrange("p (k c) -> p k c", c=P), func=AF.Copy),
            [('ps', bk)], [(dkey, t)])
        yield (0.6, 0.0)

    def proj_tm(src, skey, chunks, scr0, scrkey, b, th, after_tp=None):
        for tp in range(2):
            bks = [bank(th) for _ in range(4)]
            n = len(chunks)
            for ci, (c, sidx_) in enumerate(chunks):
                s = ring_next(scr0 + sidx_, scrkey(sidx_) if callable(scrkey) else scrkey)
                for tt in range(2):
                    t = tp * 2 + tt
                    for h in range(2):
                        bk = bks[tt * 2 + h]
                        mm(psb[bk][:, :], src[:, c, t * P:(t + 1) * P], ring[:, s, h * 512:(h + 1) * 512],
                           ci == 0, ci == n - 1, [(skey, t), ('ring', s)], [('ps', bk)])
                yield (0.87, 0.0)
            for tt in range(2):
                t = tp * 2 + tt
                sl = (b % 2) * 4 + t
                for h in range(2):
                    bk = bks[tt * 2 + h]
                    hs = slice(h * 512, (h + 1) * 512)
                    dve(lambda e, sl=sl, hs=hs, bk=bk: e.tensor_tensor(
                        out=xh[sl][:, hs], in0=xh[sl][:, hs], in1=psb[bk][:, :], op=ALU.add),
                        [('xh', sl), ('ps', bk)], [('xh', sl)])
            if after_tp is not None:
                r_ = after_tp(tp)
                if r_ is not None:
                    yield from r_

    sidx = [0]

    def mixer(b):
        pool_eng[0] = 'dve' if b <= 1 else 'pool'
        for t in range(NT):
            sl = (b % 2) * 4 + t
            yield from norm_T(sl, t, t, w1b, 'w1b', nT, 'nT', nbf, 'nbf', 'm')
        yield (0.0, 1.2)
        nT_all = [('nT', t) for t in range(NT)]
        for m in range(5):
            wd = FM_COLS[m][1]
            s = ring_next(SC_WIN + m, 'scrA', KD * wd)
            bk = bank('m')
            for k in range(KD):
                mm(psb[bk][0:wd, :], ring[:, s, k * wd:(k + 1) * wd], nT[:, k, :], k == 0, k == KD - 1,
                   nT_all + [('ring', s)], [('ps', bk)])
            if m < 2:
                act(lambda e, bk=bk, m=m: e.activation(out=qT[:, m, :], in_=psb[bk][:, :], func=AF.Copy, scale=0.125),
                    [('ps', bk)], ['qT'])
            elif m < 4:
                dve(lambda e, bk=bk, m=m: e.tensor_copy(kT[:, m - 2, :], psb[bk][:, :]), [('ps', bk)], ['kT'])
            else:
                act(lambda e, bk=bk: e.activation(out=glrT[0:16, :], in_=psb[bk][0:16, :], func=AF.Copy),
                    [('ps', bk), 'glrT'], ['glrT'])
            yield (1.73, 0.0)
        for t in range(NT):
            for (o0, wd, kind) in [(0, 512, 'v'), (512, 256, 'k')]:
                bk = bank('m')
                for k in range(KD):
                    mm(psb[bk][:, 0:wd], nT[:, k, t * P:(t + 1) * P], WINT[:, k, o0:o0 + wd], k == 0, k == KD - 1,
                       [('nT', t), 'WINT'], [('ps', bk)])
                if kind == 'v':
                    dve(lambda e, bk=bk, t=t: e.tensor_copy(v_tm[:, t, :], psb[bk][:, :]), [('ps', bk)], [('v_tm', t)])
                else:
                    dve(lambda e, bk=bk, t=t: e.tensor_copy(k_tm[:, t, :], psb[bk][:, 0:256]), [('ps', bk)], [('k_tm', t)])
                yield (1.73 if kind == 'v' else 0.9, 0.0)
        for t in range(NT):
            bk = bank('m')
            for k in range(KD):
                mm(psb[bk][:, :], nT[:, k, t * P:(t + 1) * P], WINT[:, k, 768:1280], k == 0, k == KD - 1,
                   [('nT', t), 'WINT'], [('ps', bk)])
            sigmoid_act(sq_t[:, :], psb[bk][:, :], 'sq_t', ('ps', bk))
            dve(lambda e, bk=bk, t=t: e.tensor_tensor(out=sg_tm[:, t, :], in0=sq_t[:, :], in1=psb[bk][:, :], op=ALU.mult),
                ['sq_t', ('ps', bk)], [('sg_tm', t)])
            yield (1.73, 0.0)
        for g in range(4):
            s = ring_next(SC_WIN + 5 + g, 'scrA')
            bk = bank('m')
            for k in range(KD):
                mm(psb[bk][:, :], ring[:, s, k * P:(k + 1) * P], nT[:, k, :], k == 0, k == KD - 1,
                   nT_all + [('ring', s)], [('ps', bk)])
            act(lambda e, bk=bk, g=g: e.activation(out=suT[:, g, :], in_=psb[bk][:, :], func=AF.Gelu),
                [('ps', bk)], [('suT', g)])
            yield (1.73, 0.0)
        for t in range(NT):
            bk = bank('m')
            for k in range(KD):
                mm(psb[bk][:, :], nT[:, k, t * P:(t + 1) * P], WINT[:, k, 1280:1792], k == 0, k == KD - 1,
                   [('nT', t), 'WINT'], [('ps', bk)])
            act(lambda e, bk=bk, t=t: e.activation(out=sv_tm[:, t, :], in_=psb[bk][:, :], func=AF.Gelu),
                [('ps', bk)], [('sv_tm', t)])
            yield (1.73, 0.0)
        if b % bps == 0:
            dve(lambda e: e.memset(Sst[:], 0.0), [], ['Sst'])
            dve(lambda e: e.memset(Sbf[sidx[0]][:], 0.0), [], [('Sbf', sidx[0])])
        for t in range(NT):
            ts = slice(t * P, (t + 1) * P)
            for g in range(4):
                gs = slice(g * P, (g + 1) * P)
                dve(lambda e, g=g, gs=gs, t=t: e.bn_stats(out=bst[:, g, :], in_=sv_tm[:, t, gs]),
                    [('sv_tm', t)], [('bst', g)])
                dve(lambda e, g=g: e.bn_aggr(out=mv[:, g, :], in_=bst[:, g, :]), [('bst', g)], [('mv', g)])
            mvk = [('mv', g) for g in range(4)]
            act(lambda e: e.activation(out=st2[:, 16:20], in_=mv[:, :, 1], func=AF.Ln, bias=eps_t[:, 0:1], scale=1.0),
                mvk + ['eps'], [('st2', 16)])
            act(lambda e: e.activation(out=rs[:, 16:20], in_=st2[:, 16:20], func=AF.Exp, scale=-0.5),
                [('st2', 16)], [('rs', 16)])
            bg = bank('m')
            mm(psb[bg][:, 0:256], glrT[0:17, ts], wgu_b[0:17, :], True, True, ['glrT', 'wgu_b'], [('ps', bg)])
            act(lambda e, bg=bg: e.activation(out=e_t[:, :], in_=psb[bg][:, 0:256], func=AF.Exp, scale=-1.0),
                [('ps', bg)], ['e_t'])
            act(lambda e: e.activation(out=l_t[:, :], in_=e_t[:, :], func=AF.Ln, bias=one_t[:, 0:1], scale=1.0),
                ['e_t', 'one'], ['l_t'])
            pool(lambda e: e.tensor_copy(lhi[:, :], l_t[:, :]), ['l_t'], ['lhi'])
            pool(lambda e: e.tensor_tensor(out=llo[:, :], in0=l_t[:, :], in1=lhi[:, :], op=ALU.subtract),
                ['l_t', 'lhi'], ['llo'])
            yield (0.15, 2.5)
            vh = vhat[t % 2]
            for g in range(4):
                gs = slice(g * P, (g + 1) * P)
                dve(lambda e, g=g, gs=gs, t=t, vh=vh: e.tensor_scalar(
                    out=vh[:, gs], in0=sv_tm[:, t, gs], scalar1=mv[:, g, 0:1], scalar2=rs[:, 16 + g:17 + g],
                    op0=ALU.subtract, op1=ALU.mult), [('sv_tm', t), ('mv', g), ('rs', 16)], [('vhat', t % 2)])
            by = bank('m')
            for p in range(2):
                ps_ = slice(p * P, (p + 1) * P)
                mm(psb[by][:, ps_], lhi[:, ps_], U4[:, 0:P], True, False, ['lhi', 'U4'], [('ps', by)])
                mm(psb[by][:, ps_], llo[:, ps_], U4[:, 0:P], False, True, ['llo', 'U4'], [('ps', by)])
            mm(psb[by][:, 256:512], SU[:, :], lhi[:, :], True, False, ['lhi', 'SU'], [('ps', by)])
            mm(psb[by][:, 256:512], SU[:, :], llo[:, :], False, True, ['llo', 'SU'], [('ps', by)])
            act(lambda e, by=by: e.activation(out=exq[:, :], in_=psb[by][:, 0:256], func=AF.Exp, scale=-1.0 / 16),
                [('ps', by)], ['exq'])
            act(lambda e, by=by: e.activation(out=exk[:, :], in_=psb[by][:, 0:256], func=AF.Exp, scale=1.0 / 16),
                [('ps', by)], ['exk'])
            act(lambda e, by=by: e.activation(out=eR[:, :], in_=psb[by][:, 256:512], func=AF.Exp, scale=-1.0 / 16),
                [('ps', by)], ['eR'])
            act(lambda e, by=by: e.activation(out=dec[:, :], in_=psb[by][:, 127:256:128], func=AF.Exp, scale=-1.0 / 16),
                [('ps', by)], ['dec'])
            for h2 in range(2):
                rw = slice(h2 * 64, h2 * 64 + 64)
                pool(lambda e, ts=ts, h2=h2, rw=rw: e.tensor_tensor(
                    out=qTdz[rw, :, h2, :], in0=qT[rw, :, ts],
                    in1=exq[rw, :].rearrange("p (a i) -> p a i", i=P), op=ALU.mult),
                    ['qT', 'exq'], ['qTd'])
            pool(lambda e, ts=ts: e.tensor_tensor(out=kTi[:, :, :], in0=kT[:, :, ts],
                                                 in1=exk[:, :].rearrange("p (a i) -> p a i", i=P), op=ALU.mult),
                ['kT', 'exk'], ['kTi'])
            pool(lambda e, t=t: e.tensor_tensor(out=kend[:, :], in0=k_tm[:, t, :], in1=eR[:, :], op=ALU.mult),
                [('k_tm', t), 'eR'], ['kend'])
            yield (0.45, 0.0)
            bq = bank('m')
            for g in range(4):
                gs = slice(g * P, (g + 1) * P)
                mm(psb[bq][:, gs], vh[:, gs], WsTm[:, gs], True, True, [('vhat', t % 2), 'WsTm'], [('ps', bq)])
            dve(lambda e, bq=bq: e.tensor_tensor(out=sgt[:, :], in0=psb[bq][:, :], in1=LW[:, :], op=ALU.mult),
                [('ps', bq), 'LW'], ['sgt'])
            pool(lambda e: e.tensor_tensor(out=sgt[:, :], in0=sgt[:, :], in1=Rc[:, :], op=ALU.add),
                ['sgt', 'Rc'], ['sgt'])
            pool(lambda e, ts=ts: e.tensor_tensor(
                out=mixT[:, 4:8, ts], in0=sgt[:, :].rearrange("p (g i) -> p g i", i=P), in1=suT[:, :, ts],
                op=ALU.mult), ['sgt'] + [('suT', g) for g in range(4)], [('mixT', t)])
            yield (0.3, 2.5)
            bz = bank('m')
            for p in range(2):
                mm(psb[bz][:, p * 256:(p + 1) * 256], kTi[:, p, :], qTdz[:, p, :, :], True, True,
                   ['kTi', 'qTd'], [('ps', bz)])
            dve(lambda e, bz=bz: e.tensor_tensor(out=scT[:, :], in0=psb[bz][:, :], in1=U4[:, :], op=ALU.mult),
                [('ps', bz), 'U4'], ['scT'])
            bv = bank('m')
            for p in range(2):
                mm(psb[bv][:, p * 256:(p + 1) * 256], kend[:, p * P:(p + 1) * P],
                   v_tm[:, t, p * 256:(p + 1) * 256], True, True, ['kend', ('v_tm', t)], [('ps', bv)])
            yield (0.5, 1.2)
            bw = bank('m')
            si = sidx[0]
            for h in range(4):
                p = h // 2
                hs = slice(h * P, (h + 1) * P)
                mm(psb[bw][:, hs], scT[:, hs], v_tm[:, t, hs], True, False, ['scT', ('v_tm', t)], [('ps', bw)])
                mm(psb[bw][:, hs], qTdz[:, p, h % 2, :], Sbf[si][:, p, :], False, True,
                   ['qTd', ('Sbf', si)], [('ps', bw)])
            for p in range(2):
                for h2 in range(2):
                    rw = slice(h2 * 64, h2 * 64 + 64)
                    c0 = p * 256 + h2 * P
                    dve(lambda e, p=p, bv=bv, rw=rw, c0=c0: e.scalar_tensor_tensor(
                        out=Sst[rw, p, :], in0=Sst[rw, p, :], scalar=dec[rw, p:p + 1], in1=psb[bv][rw, c0:c0 + P],
                        op0=ALU.mult, op1=ALU.add), ['Sst', 'dec', ('ps', bv)], ['Sst'])
            sn = 1 - si
            pool(lambda e, sn=sn: e.tensor_copy(Sbf[sn][:, :, :], Sst[:, :, :]), ['Sst'], [('Sbf', sn)])
            sidx[0] = sn
            act(lambda e, bw=bw: e.activation(out=sq_t[:, :], in_=psb[bw][:, :], func=AF.Square),
                [('ps', bw)], ['sq_t'])
            dve(lambda e: e.tensor_reduce(out=st[:, 12:16], in_=sq_t[:, :].rearrange("p (h e) -> p h e", e=P),
                                          axis=AX.X, op=ALU.add), ['sq_t'], [('st', 12)])
            rstd_from(12, 1.0 / P, 4)
            for h in range(4):
                hs = slice(h * P, (h + 1) * P)
                dve(lambda e, h=h, hs=hs, bw=bw, t=t: e.scalar_tensor_tensor(
                    out=og[:, hs], in0=psb[bw][:, hs], scalar=rs[:, 12 + h:13 + h], in1=sg_tm[:, t, hs],
                    op0=ALU.mult, op1=ALU.mult), [('ps', bw), ('rs', 12), ('sg_tm', t)], ['og'])
            yield (0.7, 4.0)
            bt = bank('m')
            for h in range(4):
                hs = slice(h * P, (h + 1) * P)
                tr(psb16[bt][:, hs], og[:, hs], ['og'], [('ps', bt)])
            dve(lambda e, bt=bt, ts=ts: e.tensor_scalar(
                out=mixT[:, 0:4, ts], in0=psb16[bt][:, 0:512].rearrange("p (h i) -> p h i", i=P),
                scalar1=glaw_t[:, 0:1], scalar2=None, op0=ALU.mult), [('ps', bt), 'glaw'], [('mixT', t)])
            yield (0.3, 0.0)
        def norm2(tp):
            yield 'need_gu_done'
            for tt in range(2):
                t = tp * 2 + tt
                yield from norm_T((b % 2) * 4 + t, t, 4 + t, w2b, 'w2b', n2T, 'n2T', nbf2, 'nbf2', 'm')

        yield from proj_tm(mixT, 'mixT', [(c, c) for c in range(8)], SC_WOUT, 'scrB', b, 'm', after_tp=norm2)

    def ffn(b):
        n2_all = [('n2T', t) for t in range(NT)]

        def finish(tp):
            for tt in range(2):
                t = tp * 2 + tt
                sl = (b % 2) * 4 + t
                col = 8 + t
                act(lambda e, sl=sl, col=col: e.activation(out=junk[:, :], in_=xh[sl][:, :], func=AF.Square,
                                                           accum_out=st[:, col:col + 1]), [('xh', sl)], [('st', col)])
                rstd_from(col, 1.0 / D)
                dve(lambda e, sl=sl, col=col: e.scalar_tensor_tensor(
                    out=xh[sl][:, :], in0=xh[sl][:, :], scalar=rs[:, col:col + 1], in1=wfb[:, :],
                    op0=ALU.mult, op1=ALU.mult), [('xh', sl), ('rs', col), 'wfb'], [('xh', sl)])
                r0 = b * TB + t * P
                pdma(out[r0:r0 + P, :], xh[sl][:, :], [('xh', sl)], [('out', b, t)], ('xs', sl))
            if b + 2 < nblk:
                load_x(b + 2, [tp * 2, tp * 2 + 1])

        for pas in range(2):
            for jj in range(11):
                j = pas * 11 + jj
                bks = []
                for gu in range(2):
                    s = ring_next(SC_GU + 2 * j + gu, 'scrC%d' % (j // 3))
                    bk = bank('f')
                    bks.append(bk)
                    for k in range(KD):
                        mm(psb[bk][:, :], ring[:, s, k * P:(k + 1) * P], n2T[:, k, :], k == 0, k == KD - 1,
                           n2_all + [('ring', s)], [('ps', bk)])
                    yield (1.73, 0.0)
                tf = tmpF[jj % 2]
                tk = ('tmpF', jj % 2)
                sigmoid_act(tf[:, :], psb[bks[0]][:, :], tk, ('ps', bks[0]))
                dve(lambda e, bk=bks[0], tf=tf: e.tensor_tensor(out=tf[:, :], in0=tf[:, :], in1=psb[bk][:, :], op=ALU.mult),
                    [tk, ('ps', bks[0])], [tk])
                dve(lambda e, bk=bks[1], tf=tf, jj=jj: e.tensor_tensor(
                    out=gT[:, jj, :], in0=tf[:, :], in1=psb[bk][:, :], op=ALU.mult),
                    [tk, ('ps', bks[1])], [('gT', t_) for t_ in range(NT)])
            if pas == 1:
                yield 'gu_done'
            yield (0.0, 3.0)
            yield from proj_tm(gT, 'gT', [(jj, pas * 11 + jj) for jj in range(11)], SC_D, (lambda j_: 'scrD%d' % (j_ // 6)), b, 'f',
                               after_tp=(finish if pas == 1 else None))

    def run_alone(g):
        for _ in g:
            pass

    def interleave(ga, gb):
        tnow = 0.0
        rdy = [0.0, 0.0]
        done = [False, False]
        gens = [ga, gb]
        gu_done = False
        blocked = False
        while not (done[0] and done[1]):
            cands = [i for i in (0, 1) if not done[i] and not (i == 1 and blocked and not gu_done)]
            i = min(cands, key=lambda i_: (max(rdy[i_], tnow), -i_))
            try:
                y = next(gens[i])
            except StopIteration:
                done[i] = True
                if i == 0:
                    gu_done = True
                continue
            if y == 'gu_done':
                gu_done = True
                continue
            if y == 'need_gu_done':
                blocked = True
                continue
            pe_c, lat = y
            lat = lat * LATSCALE
            tnow = max(tnow, rdy[i]) + pe_c
            rdy[i] = tnow + lat

    run_alone(mixer(0))
    for b in range(nblk):
        if b + 1 < nblk:
            interleave(ffn(b), mixer(b + 1))
        else:
            run_alone(ffn(b))
    S.add('pool', None, [('out', b, t) for b in range(nblk) for t in range(NT)], [])

    with nc.allow_low_precision(reason="bf16 matmul operands, fp32 accumulate"):
        with nc.allow_non_contiguous_dma(reason="weight re-layout"):
            S.emit(nc)
    return nc


_CACHE = {}


def _consts():
    i = np.arange(P)
    U = (i[:, None] <= i[None, :]).astype(np.float32)
    SUm = (i[:, None] > i[None, :]).astype(np.float32)
    bf = ml_dtypes.bfloat16
    return dict(c_ident=np.eye(P, dtype=np.float32).astype(bf),
                c_u4=np.tile(U, (1, 4)).astype(bf),
                c_su=SUm.astype(bf))


def prep_shared(inp):
    f = lambda a: np.ascontiguousarray(np.asarray(a, dtype=np.float32))
    d = dict(
        w_in=f(inp["w_in"][0]), w_out=f(inp["w_out"][0]), w_g=f(inp["w_ffn_gate"][0]),
        w_u=f(inp["w_ffn_up"][0]), w_d=f(inp["w_ffn_down"][0]),
        n1b=f(np.broadcast_to(np.asarray(inp["norm1_w"][0]), (P, D))),
        n2b=f(np.broadcast_to(np.asarray(inp["norm2_w"][0]), (P, D))),
        nfb=f(np.broadcast_to(np.asarray(inp["final_norm_w"]), (P, D))),
        wgu=f(np.concatenate([np.asarray(inp["w_gate_up"][0]), np.asarray(inp["b_gate_up"][0])[None, :]], axis=0)),
        glaw=f(np.asarray(inp["gla_norm_w"][0]).reshape(P, 1)),
        lnw=f(np.asarray(inp["sg_ln_w"][0]).T), lnb=f(np.asarray(inp["sg_ln_b"][0]).T),
        wsT=f(np.asarray(inp["sg_w_s"][0]).transpose(2, 0, 1).reshape(P, 512)),
        bsb=f(np.broadcast_to(np.asarray(inp["sg_b_s"][0]).reshape(1, 512), (P, 512))),
    )
    d.update(_consts())
    return d


def kernel(**inputs):
    x = np.asarray(inputs["x"], dtype=np.float32)
    B, T, _ = x.shape
    n_tok = (B // NCORES) * T
    key = (n_tok, T)
    if key not in _CACHE:
        _CACHE[key] = build(n_tok, T)
    nc = _CACHE[key]
    shared = prep_shared(inputs)
    xs = x.reshape(NCORES, n_tok, D)
    in_maps = []
    for c in range(NCORES):
        m = dict(shared)
        m["x"] = np.ascontiguousarray(xs[c])
        in_maps.append(m)
    res = run_bass_kernel_spmd(nc, in_maps, core_ids=list(range(NCORES)))
    outs = [np.asarray(r["out"], dtype=np.float32) for r in res.results]
    return np.stack(outs, axis=0).reshape(B, T, D)
```

```python
import numpy as np
import ml_dtypes
import concourse.bass as bass
import concourse.mybir as mybir
from concourse.bass_utils import run_bass_kernel_spmd

F32 = mybir.dt.float32
BF16 = mybir.dt.bfloat16
AF = mybir.ActivationFunctionType
ALU = mybir.AluOpType
AX = mybir.AxisListType

P = 128
D = 1024
KD = 8
TB = 512
NT = 4
DFF = 2816
NF = 22
NCORES = 8
EPS = 1e-5
INC = 2576
NR = 12
import os as _os
LATSCALE = float(_os.environ.get('LATSCALE', '1.0'))

SC_WIN = 0
SC_WOUT = 9
SC_GU = 17
SC_D = 61
SC_N = 83
FM_COLS = [(0, 128), (128, 128), (256, 128), (384, 128), (1024, 16),
           (1552, 128), (1680, 128), (1808, 128), (1936, 128)]


class Sched:
    def __init__(self):
        self.ops = []
        self.last_w = {}
        self.readers = {}
        self.dma_cnt = {}

    def add(self, eng, fn, r=(), w=(), dma=None):
        i = len(self.ops)
        deps = set()
        for k in r:
            if k in self.last_w:
                deps.add(self.last_w[k])
        for k in w:
            if k in self.last_w:
                deps.add(self.last_w[k])
            deps.update(self.readers.get(k, ()))
        for k in r:
            if fn is not None:
                self.readers.setdefault(k, []).append(i)
        for k in w:
            self.last_w[k] = i
            self.readers[k] = []
        deps.discard(i)
        op = dict(eng=eng, fn=fn, r=frozenset(r), w=frozenset(w), deps=deps, dma=dma,
                  sig=None, need=False)
        if dma is not None:
            n = self.dma_cnt.get(dma, 0) + 1
            self.dma_cnt[dma] = n
            op['sig'] = (dma, 16 * n)
        self.ops.append(op)
        return i

    def finalize(self):
        ops = self.ops
        for op in ops:
            keep = []
            for d in op['deps']:
                p = ops[d]
                if p['dma'] is None and op['dma'] is None and p['eng'] == op['eng']:
                    if op['eng'] == 'pe':
                        continue
                    if not (p['w'] & op['r']):
                        continue
                keep.append(d)
                p['need'] = True
            op['deps'] = keep
        cnt = {}
        for op in ops:
            if op['dma'] is None and op['need']:
                c = cnt.get(op['eng'], 0) + 1
                cnt[op['eng']] = c
                op['sig'] = (op['eng'], c)

    def emit(self, nc):
        self.finalize()
        ops = self.ops
        keys = []
        for op in ops:
            if op['sig'] is not None and op['sig'][0] not in keys:
                keys.append(op['sig'][0])
        sems = {k: nc.alloc_semaphore("s_%d" % i) for i, k in enumerate(keys)}
        engmap = {'pe': 'tensor', 'act': 'scalar', 'dve': 'vector', 'pool': 'gpsimd', 'sp': 'sync'}
        with nc.Block() as block:
            for eng, attr in engmap.items():
                mine = [op for op in ops if op['eng'] == eng]

                def body(e, mine=mine):
                    seen = {}
                    for op in mine:
                        need = {}
                        for d in op['deps']:
                            s, v = ops[d]['sig']
                            if seen.get(s, 0) < v and need.get(s, 0) < v:
                                need[s] = v
                        for s, v in need.items():
                            e.wait_ge(sems[s], v)
                            seen[s] = v
                        if op['fn'] is None:
                            continue
                        ins = op['fn'](e)
                        if op['sig'] is not None:
                            ins.then_inc(sems[op['sig'][0]], 16 if op['dma'] is not None else 1)

                getattr(block, attr)(body)


def build(n_tok, seq_len, stop=None):
    nblk = n_tok // TB
    bps = seq_len // TB
    nc = bass.Bass("TRN2", target_bir_lowering=False)
    S = Sched()

    def din(name, shape, dt=F32):
        return nc.dram_tensor(name, list(shape), dt, kind="ExternalInput").ap()

    x = din("x", [n_tok, D])
    out = nc.dram_tensor("out", [n_tok, D], F32, kind="ExternalOutput").ap()
    w_in = din("w_in", [D, INC])
    w_out = din("w_out", [D, D])
    w_g = din("w_g", [D, DFF])
    w_u = din("w_u", [D, DFF])
    w_d = din("w_d", [DFF, D])
    n1b = din("n1b", [P, D])
    n2b = din("n2b", [P, D])
    nfb = din("nfb", [P, D])
    wgu = din("wgu", [17, 256])
    glaw = din("glaw", [P, 1])
    lnw = din("lnw", [P, 4])
    lnb = din("lnb", [P, 4])
    wsT = din("wsT", [P, 512])
    bsb = din("bsb", [P, 512])
    c_ident = din("c_ident", [P, P], BF16)
    c_u4 = din("c_u4", [P, 512], BF16)
    c_su = din("c_su", [P, P], BF16)
    wscr = nc.dram_tensor("wscr", [SC_N, P, 1024], BF16, kind="Internal").ap()

    def sb(name, shape, dt):
        return nc.alloc_sbuf_tensor(name, list(shape), dt)

    xh = [sb("xh%d" % i, [P, D], F32) for i in range(8)]
    w1b = sb("w1b", [P, D], F32)
    w2b = sb("w2b", [P, D], F32)
    wfb = sb("wfb", [P, D], F32)
    WINT = sb("WINT", [P, KD, 1792], BF16)
    ring = sb("ring", [P, NR, 1024], BF16)
    nbf = sb("nbf", [P, D], BF16)
    nbf2 = sb("nbf2", [P, D], BF16)
    junk = sb("junk", [P, D], BF16)
    nT = sb("nT", [P, KD, TB], BF16)
    n2T = sb("n2T", [P, KD, TB], BF16)
    qT = sb("qT", [P, 2, TB], F32)
    kT = sb("kT", [P, 2, TB], F32)
    glrT = sb("glrT", [32, TB], BF16)
    v_tm = sb("v_tm", [P, NT, 512], BF16)
    k_tm = sb("k_tm", [P, NT, 256], F32)
    sg_tm = sb("sg_tm", [P, NT, 512], BF16)
    suT = sb("suT", [P, 4, TB], BF16)
    sv_tm = sb("sv_tm", [P, NT, 512], BF16)
    vhat = [sb("vhat%d" % i, [P, 512], BF16) for i in range(2)]
    e_t = sb("e_t", [P, 256], F32)
    l_t = sb("l_t", [P, 256], F32)
    lhi = sb("lhi", [P, 256], BF16)
    llo = sb("llo", [P, 256], BF16)
    exq = sb("exq", [P, 256], F32)
    exk = sb("exk", [P, 256], F32)
    eR = sb("eR", [P, 256], F32)
    dec = sb("dec", [P, 2], F32)
    qTdz = sb("qTdz", [P, 2, 2, P], BF16)
    kTi = sb("kTi", [P, 2, P], BF16)
    kend = sb("kend", [P, 256], BF16)
    scT = sb("scT", [P, 512], BF16)
    sq_t = sb("sq_t", [P, 512], F32)
    og = sb("og", [P, 512], BF16)
    Sst = sb("Sst", [P, 2, P], F32)
    Sbf = [sb("Sbf%d" % i, [P, 2, P], BF16) for i in range(2)]
    sgt = sb("sgt", [P, 512], F32)
    mixT = sb("mixT", [P, KD, TB], BF16)
    gT = sb("gT", [P, 11, TB], BF16)
    tmpF = [sb("tmpF%d" % i, [P, TB], F32) for i in range(2)]
    ident = sb("ident", [P, P], BF16)
    U4 = sb("U4", [P, 512], BF16)
    SU = sb("SU", [P, P], BF16)
    ones_bf = sb("ones_bf", [P, P], BF16)
    ones_f = sb("ones_f", [P, P], F32)
    WsTm = sb("WsTm", [P, 512], BF16)
    wsT_f = sb("wsT_f", [P, 512], F32)
    bsb_t = sb("bsb_t", [P, 512], F32)
    Rc = sb("Rc", [P, 512], F32)
    LW = sb("LW", [P, 512], F32)
    lnw_t = sb("lnw_t", [P, 4], F32)
    lnb_t = sb("lnb_t", [P, 4], F32)
    glaw_t = sb("glaw_t", [P, 1], F32)
    wgu_f = sb("wgu_f", [32, 256], F32)
    wgu_b = sb("wgu_b", [32, 256], BF16)
    eps_t = sb("eps_t", [P, 1], F32)
    one_t = sb("one_t", [P, 1], F32)
    st = sb("st", [P, 24], F32)
    st2 = sb("st2", [P, 24], F32)
    rs = sb("rs", [P, 24], F32)
    bst = sb("bst", [P, 4, 6], F32)
    mv = sb("mv", [P, 4, 2], F32)

    psb = [nc.alloc_psum_tensor("ps%d" % i, [P, 512], F32) for i in range(8)]
    psb16 = [t.bitcast(BF16) for t in psb]
    state = dict(bank=0, rpos=0)

    bstate = {'m': 0, 'f': 0}

    def bank(th='m'):
        b = bstate[th]
        bstate[th] = (b + 1) % 4
        return b + (0 if th == 'm' else 4)

    pe = lambda fn, r, w: S.add('pe', fn, r, w)
    act = lambda fn, r, w: S.add('act', fn, r, w)
    dve = lambda fn, r, w: S.add('dve', fn, r, w)
    pool_eng = ['dve']
    pool = lambda fn, r, w: S.add(pool_eng[0], fn, r, w)

    def mm(o, l, rh, st_, sp_, r, w):
        pe(lambda e: e.matmul(o, l, rh, start=st_, stop=sp_), r, w)

    def tr(o, i_, r, w):
        pe(lambda e: e.transpose(o, i_, ident[:, :]), r + ['ident'], w)

    npd = [0]

    def pdma(o, i_, r, w, key):
        S.add('pool', lambda e: e.dma_start(out=o, in_=i_), r, w, dma=key)
        if key[0] == 'g':
            npd[0] += 1
            if npd[0] % 16 == 0:
                S.add('pool', None, list(w), [])

    def sdma(o, i_, r, w, key):
        S.add('sp', lambda e: e.dma_start(out=o, in_=i_), r, w, dma=key)

    for (t_, d_, nm) in [(ident, c_ident, 'ident'), (U4, c_u4, 'U4'), (SU, c_su, 'SU'),
                         (w1b, n1b, 'w1b'), (w2b, n2b, 'w2b'), (wfb, nfb, 'wfb'),
                         (lnw_t, lnw, 'lnw'), (lnb_t, lnb, 'lnb'), (glaw_t, glaw, 'glaw'),
                         (wsT_f, wsT, 'wsT_f'), (bsb_t, bsb, 'bsb')]:
        sdma(t_[:], d_[:], [], [nm], ('c', nm))
    sdma(wgu_f[0:17, :], wgu[:, :], [], ['wgu_f'], ('c', 'wgu'))

    for t in range(NT):
        S.add('pool', (lambda e, t=t: e.dma_start(out=xh[t][:, :], in_=x[t * P:(t + 1) * P, :])), [], [('xh', t)], dma=('xs', t))
    w_in_v = w_in.rearrange("(k p) c -> p k c", p=P)

    def cast_fm(m):
        c0, wd = FM_COLS[m]
        pdma(wscr[SC_WIN + m][:, 0:KD * wd].rearrange("p (k c) -> p k c", c=wd),
             w_in_v[:, :, c0:c0 + wd], [], ['scrA%d' % (m // 5)], ('g', 'scrA%d' % (m // 5)))

    def cast_wint(c0, wd, o0, nm):
        pdma(WINT[:, :, o0:o0 + wd], w_in_v[:, :, c0:c0 + wd], [], ['WINT_' + nm], ('g', 'wint_' + nm))

    for m in range(5):
        cast_fm(m)
    cast_wint(512, 512, 0, 'v')
    cast_wint(256, 256, 512, 'k')
    cast_wint(1040, 512, 768, 'g')
    for m in range(5, 9):
        cast_fm(m)
    cast_wint(2064, 512, 1280, 's')
    if nblk > 1:
        for t in range(NT):
            S.add('pool', (lambda e, t=t: e.dma_start(out=xh[4 + t][:, :], in_=x[TB + t * P:TB + (t + 1) * P, :])),
                  [], [('xh', 4 + t)], dma=('xs', 4 + t))
    pdma(wscr[SC_WOUT:SC_WOUT + 8], w_out.rearrange("(c p) d -> c p d", p=P), [], ['scrB'], ('g', 'scrB'))
    w_g_v = w_g.rearrange("(k p) f -> p k f", p=P)
    w_u_v = w_u.rearrange("(k p) f -> p k f", p=P)
    w_d_v = w_d.rearrange("(j p) d -> j p d", p=P)
    for j in range(NF):
        for gu, wv in enumerate((w_g_v, w_u_v)):
            pdma(wscr[SC_GU + 2 * j + gu].rearrange("p (k f) -> p k f", f=P),
                 wv[:, :, j * P:(j + 1) * P], [], ['scrC%d' % (j // 3)], ('g', 'scrC%d' % (j // 3)))
        if j % 6 == 5 or j == NF - 1:
            j0 = (j // 6) * 6
            pdma(wscr[SC_D + j0:SC_D + j + 1], w_d_v[j0:j + 1], [], ['scrD%d' % (j // 6)], ('g', 'scrD%d' % (j // 6)))

    dve(lambda e: e.memset(eps_t[:], EPS), [], ['eps'])
    dve(lambda e: e.memset(one_t[:], 1.0), [], ['one'])
    dve(lambda e: e.memset(ones_bf[:], 1.0), [], ['ones_bf'])
    dve(lambda e: e.memset(ones_f[:], 1.0), [], ['ones_f'])
    dve(lambda e: e.memset(glrT[:], 1.0), [], ['glrT'])
    dve(lambda e: e.memset(qTdz[:], 0.0), [], ['qTd'])
    dve(lambda e: e.tensor_copy(wgu_b[0:17, :], wgu_f[0:17, :]), ['wgu_f'], ['wgu_b'])
    dve(lambda e: e.tensor_tensor(out=WsTm[:], in0=wsT_f[:], in1=U4[:], op=ALU.mult),
        ['wsT_f', 'U4'], ['WsTm'])
    b0 = bank()
    mm(psb[b0][:, :], ones_bf[:, :], WsTm[:, :], True, True, ['ones_bf', 'WsTm'], [('ps', b0)])
    for g in range(4):
        gs = slice(g * P, (g + 1) * P)
        dve(lambda e, g=g, gs=gs: e.scalar_tensor_tensor(
            out=Rc[:, gs], in0=psb[b0][:, gs], scalar=lnb_t[:, g:g + 1], in1=bsb_t[:, gs],
            op0=ALU.mult, op1=ALU.add), [('ps', b0), 'lnb', 'bsb'], ['Rc'])
        dve(lambda e, g=g, gs=gs: e.tensor_scalar(
            out=LW[:, gs], in0=ones_f[:, :], scalar1=lnw_t[:, g:g + 1], scalar2=None, op0=ALU.mult),
            ['ones_f', 'lnw'], ['LW'])

    def ring_next(idx, key, width=1024):
        s = state['rpos'] % NR
        state['rpos'] += 1
        sdma(ring[:, s, 0:width], wscr[idx][:, 0:width], [key], [('ring', s)], ('ring', s))
        return s

    def load_x(b, tiles):
        for t in tiles:
            sl = (b % 2) * 4 + t
            r0 = b * TB + t * P
            pdma(xh[sl][:, :], x[r0:r0 + P, :], [], [('xh', sl)], ('xs', sl))

    def rstd_from(col, scale, n=1):
        cs = slice(col, col + n)
        act(lambda e: e.activation(out=st2[:, cs], in_=st[:, cs], func=AF.Ln, bias=eps_t[:, 0:1], scale=scale),
            [('st', col), 'eps'], [('st2', col)])
        act(lambda e: e.activation(out=rs[:, cs], in_=st2[:, cs], func=AF.Exp, scale=-0.5),
            [('st2', col)], [('rs', col)])

    def sigmoid_act(dst, src, dkey, skey):
        act(lambda e: e.activation(out=dst, in_=src, func=AF.Exp, scale=-1.0), [skey], [dkey])
        act(lambda e: e.activation(out=dst, in_=dst, func=AF.Ln, bias=one_t[:, 0:1], scale=1.0), [dkey, 'one'], [dkey])
        act(lambda e: e.activation(out=dst, in_=dst, func=AF.Exp, scale=-1.0), [dkey], [dkey])

    def norm_T(sl, t, col, wb, wbk, dstT, dkey, nb_, nbk, th):
        act(lambda e: e.activation(out=junk[:, :], in_=xh[sl][:, :], func=AF.Square,
                                   accum_out=st[:, col:col + 1]), [('xh', sl)], [('st', col)])
        rstd_from(col, 1.0 / D)
        dve(lambda e: e.scalar_tensor_tensor(out=nb_[:, :], in0=xh[sl][:, :], scalar=rs[:, col:col + 1],
                                             in1=wb[:, :], op0=ALU.mult, op1=ALU.mult),
            [('xh', sl), ('rs', col), wbk], [nbk])
        yield (0.0, 4.0)
        bk = bank(th)
        for k in range(KD):
            tr(psb16[bk][:, k * P:(k + 1) * P], nb_[:, k * P:(k + 1) * P], [nbk], [('ps', bk)])
        act(lambda e: e.activation(out=dstT[:, :, t * P:(t + 1) * P],
                                   in_=psb16[bk][:, :].rearrange("p (k c) -> p k c", c=P), func=AF.Copy),
            [('ps', bk)], [(dkey, t)])
        yield (0.6, 0.0)

    def proj_tm(src, skey, chunks, scr0, scrkey, b, th, after_tp=None):
        for tp in range(2):
            bks = [bank(th) for _ in range(4)]
            n = len(chunks)
            for ci, (c, sidx_) in enumerate(chunks):
                s = ring_next(scr0 + sidx_, scrkey(sidx_) if callable(scrkey) else scrkey)
                for tt in range(2):
                    t = tp * 2 + tt
                    for h in range(2):
                        bk = bks[tt * 2 + h]
                        mm(psb[bk][:, :], src[:, c, t * P:(t + 1) * P], ring[:, s, h * 512:(h + 1) * 512],
                           ci == 0, ci == n - 1, [(skey, t), ('ring', s)], [('ps', bk)])
                yield (0.87, 0.0)
            for tt in range(2):
                t = tp * 2 + tt
                sl = (b % 2) * 4 + t
                for h in range(2):
                    bk = bks[tt * 2 + h]
                    hs = slice(h * 512, (h + 1) * 512)
                    dve(lambda e, sl=sl, hs=hs, bk=bk: e.tensor_tensor(
                        out=xh[sl][:, hs], in0=xh[sl][:, hs], in1=psb[bk][:, :], op=ALU.add),
                        [('xh', sl), ('ps', bk)], [('xh', sl)])
            if after_tp is not None:
                r_ = after_tp(tp)
                if r_ is not None:
                    yield from r_

    sidx = [0]

    def mixer(b):
        pool_eng[0] = 'dve' if b <= 1 else 'pool'
        for t in range(NT):
            sl = (b % 2) * 4 + t
            yield from norm_T(sl, t, t, w1b, 'w1b', nT, 'nT', nbf, 'nbf', 'm')
        yield (0.0, 1.2)
        nT_all = [('nT', t) for t in range(NT)]
        for m in range(5):
            wd = FM_COLS[m][1]
            s = ring_next(SC_WIN + m, 'scrA0', KD * wd)
            bk = bank('m')
            for k in range(KD):
                mm(psb[bk][0:wd, :], ring[:, s, k * wd:(k + 1) * wd], nT[:, k, :], k == 0, k == KD - 1,
                   nT_all + [('ring', s)], [('ps', bk)])
            if m < 2:
                act(lambda e, bk=bk, m=m: e.activation(out=qT[:, m, :], in_=psb[bk][:, :], func=AF.Copy, scale=0.125),
                    [('ps', bk)], ['qT'])
            elif m < 4:
                dve(lambda e, bk=bk, m=m: e.tensor_copy(kT[:, m - 2, :], psb[bk][:, :]), [('ps', bk)], ['kT'])
            else:
                act(lambda e, bk=bk: e.activation(out=glrT[0:16, :], in_=psb[bk][0:16, :], func=AF.Copy),
                    [('ps', bk), 'glrT'], ['glrT'])
            yield (1.73, 0.0)
        for t in range(NT):
            for (o0, wd, kind) in [(0, 512, 'v'), (512, 256, 'k')]:
                bk = bank('m')
                for k in range(KD):
                    mm(psb[bk][:, 0:wd], nT[:, k, t * P:(t + 1) * P], WINT[:, k, o0:o0 + wd], k == 0, k == KD - 1,
                       [('nT', t), 'WINT_' + kind], [('ps', bk)])
                if kind == 'v':
                    dve(lambda e, bk=bk, t=t: e.tensor_copy(v_tm[:, t, :], psb[bk][:, :]), [('ps', bk)], [('v_tm', t)])
                else:
                    dve(lambda e, bk=bk, t=t: e.tensor_copy(k_tm[:, t, :], psb[bk][:, 0:256]), [('ps', bk)], [('k_tm', t)])
                yield (1.73 if kind == 'v' else 0.9, 0.0)
        for t in range(NT):
            bk = bank('m')
            for k in range(KD):
                mm(psb[bk][:, :], nT[:, k, t * P:(t + 1) * P], WINT[:, k, 768:1280], k == 0, k == KD - 1,
                   [('nT', t), 'WINT_g'], [('ps', bk)])
            sigmoid_act(sq_t[:, :], psb[bk][:, :], 'sq_t', ('ps', bk))
            dve(lambda e, bk=bk, t=t: e.tensor_tensor(out=sg_tm[:, t, :], in0=sq_t[:, :], in1=psb[bk][:, :], op=ALU.mult),
                ['sq_t', ('ps', bk)], [('sg_tm', t)])
            yield (1.73, 0.0)
        for g in range(4):
            s = ring_next(SC_WIN + 5 + g, 'scrA1')
            bk = bank('m')
            for k in range(KD):
                mm(psb[bk][:, :], ring[:, s, k * P:(k + 1) * P], nT[:, k, :], k == 0, k == KD - 1,
                   nT_all + [('ring', s)], [('ps', bk)])
            act(lambda e, bk=bk, g=g: e.activation(out=suT[:, g, :], in_=psb[bk][:, :], func=AF.Gelu),
                [('ps', bk)], [('suT', g)])
            yield (1.73, 0.0)
        for t in range(NT):
            bk = bank('m')
            for k in range(KD):
                mm(psb[bk][:, :], nT[:, k, t * P:(t + 1) * P], WINT[:, k, 1280:1792], k == 0, k == KD - 1,
                   [('nT', t), 'WINT_s'], [('ps', bk)])
            act(lambda e, bk=bk, t=t: e.activation(out=sv_tm[:, t, :], in_=psb[bk][:, :], func=AF.Gelu),
                [('ps', bk)], [('sv_tm', t)])
            yield (1.73, 0.0)
        if b % bps == 0:
            dve(lambda e: e.memset(Sst[:], 0.0), [], ['Sst'])
            dve(lambda e: e.memset(Sbf[sidx[0]][:], 0.0), [], [('Sbf', sidx[0])])
        for t in range(NT):
            ts = slice(t * P, (t + 1) * P)
            for g in range(4):
                gs = slice(g * P, (g + 1) * P)
                dve(lambda e, g=g, gs=gs, t=t: e.bn_stats(out=bst[:, g, :], in_=sv_tm[:, t, gs]),
                    [('sv_tm', t)], [('bst', g)])
                dve(lambda e, g=g: e.bn_aggr(out=mv[:, g, :], in_=bst[:, g, :]), [('bst', g)], [('mv', g)])
            mvk = [('mv', g) for g in range(4)]
            act(lambda e: e.activation(out=st2[:, 16:20], in_=mv[:, :, 1], func=AF.Ln, bias=eps_t[:, 0:1], scale=1.0),
                mvk + ['eps'], [('st2', 16)])
            act(lambda e: e.activation(out=rs[:, 16:20], in_=st2[:, 16:20], func=AF.Exp, scale=-0.5),
                [('st2', 16)], [('rs', 16)])
            bg = bank('m')
            mm(psb[bg][:, 0:256], glrT[0:17, ts], wgu_b[0:17, :], True, True, ['glrT', 'wgu_b'], [('ps', bg)])
            act(lambda e, bg=bg: e.activation(out=e_t[:, :], in_=psb[bg][:, 0:256], func=AF.Exp, scale=-1.0),
                [('ps', bg)], ['e_t'])
            act(lambda e: e.activation(out=l_t[:, :], in_=e_t[:, :], func=AF.Ln, bias=one_t[:, 0:1], scale=1.0),
                ['e_t', 'one'], ['l_t'])
            pool(lambda e: e.tensor_copy(lhi[:, :], l_t[:, :]), ['l_t'], ['lhi'])
            pool(lambda e: e.tensor_tensor(out=llo[:, :], in0=l_t[:, :], in1=lhi[:, :], op=ALU.subtract),
                ['l_t', 'lhi'], ['llo'])
            yield (0.15, 2.5)
            vh = vhat[t % 2]
            for g in range(4):
                gs = slice(g * P, (g + 1) * P)
                dve(lambda e, g=g, gs=gs, t=t, vh=vh: e.tensor_scalar(
                    out=vh[:, gs], in0=sv_tm[:, t, gs], scalar1=mv[:, g, 0:1], scalar2=rs[:, 16 + g:17 + g],
                    op0=ALU.subtract, op1=ALU.mult), [('sv_tm', t), ('mv', g), ('rs', 16)], [('vhat', t % 2)])
            by = bank('m')
            for p in range(2):
                ps_ = slice(p * P, (p + 1) * P)
                mm(psb[by][:, ps_], lhi[:, ps_], U4[:, 0:P], True, False, ['lhi', 'U4'], [('ps', by)])
                mm(psb[by][:, ps_], llo[:, ps_], U4[:, 0:P], False, True, ['llo', 'U4'], [('ps', by)])
            mm(psb[by][:, 256:512], SU[:, :], lhi[:, :], True, False, ['lhi', 'SU'], [('ps', by)])
            mm(psb[by][:, 256:512], SU[:, :], llo[:, :], False, True, ['llo', 'SU'], [('ps', by)])
            act(lambda e, by=by: e.activation(out=exq[:, :], in_=psb[by][:, 0:256], func=AF.Exp, scale=-1.0 / 16),
                [('ps', by)], ['exq'])
            act(lambda e, by=by: e.activation(out=exk[:, :], in_=psb[by][:, 0:256], func=AF.Exp, scale=1.0 / 16),
                [('ps', by)], ['exk'])
            act(lambda e, by=by: e.activation(out=eR[:, :], in_=psb[by][:, 256:512], func=AF.Exp, scale=-1.0 / 16),
                [('ps', by)], ['eR'])
            act(lambda e, by=by: e.activation(out=dec[:, :], in_=psb[by][:, 127:256:128], func=AF.Exp, scale=-1.0 / 16),
                [('ps', by)], ['dec'])
            for h2 in range(2):
                rw = slice(h2 * 64, h2 * 64 + 64)
                pool(lambda e, ts=ts, h2=h2, rw=rw: e.tensor_tensor(
                    out=qTdz[rw, :, h2, :], in0=qT[rw, :, ts],
                    in1=exq[rw, :].rearrange("p (a i) -> p a i", i=P), op=ALU.mult),
                    ['qT', 'exq'], ['qTd'])
            pool(lambda e, ts=ts: e.tensor_tensor(out=kTi[:, :, :], in0=kT[:, :, ts],
                                                 in1=exk[:, :].rearrange("p (a i) -> p a i", i=P), op=ALU.mult),
                ['kT', 'exk'], ['kTi'])
            pool(lambda e, t=t: e.tensor_tensor(out=kend[:, :], in0=k_tm[:, t, :], in1=eR[:, :], op=ALU.mult),
                [('k_tm', t), 'eR'], ['kend'])
            yield (0.45, 0.0)
            bq = bank('m')
            for g in range(4):
                gs = slice(g * P, (g + 1) * P)
                mm(psb[bq][:, gs], vh[:, gs], WsTm[:, gs], True, True, [('vhat', t % 2), 'WsTm'], [('ps', bq)])
            dve(lambda e, bq=bq: e.tensor_tensor(out=sgt[:, :], in0=psb[bq][:, :], in1=LW[:, :], op=ALU.mult),
                [('ps', bq), 'LW'], ['sgt'])
            pool(lambda e: e.tensor_tensor(out=sgt[:, :], in0=sgt[:, :], in1=Rc[:, :], op=ALU.add),
                ['sgt', 'Rc'], ['sgt'])
            pool(lambda e, ts=ts: e.tensor_tensor(
                out=mixT[:, 4:8, ts], in0=sgt[:, :].rearrange("p (g i) -> p g i", i=P), in1=suT[:, :, ts],
                op=ALU.mult), ['sgt'] + [('suT', g) for g in range(4)], [('mixT', t)])
            yield (0.3, 2.5)
            bz = bank('m')
            for p in range(2):
                mm(psb[bz][:, p * 256:(p + 1) * 256], kTi[:, p, :], qTdz[:, p, :, :], True, True,
                   ['kTi', 'qTd'], [('ps', bz)])
            dve(lambda e, bz=bz: e.tensor_tensor(out=scT[:, :], in0=psb[bz][:, :], in1=U4[:, :], op=ALU.mult),
                [('ps', bz), 'U4'], ['scT'])
            bv = bank('m')
            for p in range(2):
                mm(psb[bv][:, p * 256:(p + 1) * 256], kend[:, p * P:(p + 1) * P],
                   v_tm[:, t, p * 256:(p + 1) * 256], True, True, ['kend', ('v_tm', t)], [('ps', bv)])
            yield (0.5, 1.2)
            bw = bank('m')
            si = sidx[0]
            for h in range(4):
                p = h // 2
                hs = slice(h * P, (h + 1) * P)
                mm(psb[bw][:, hs], scT[:, hs], v_tm[:, t, hs], True, False, ['scT', ('v_tm', t)], [('ps', bw)])
                mm(psb[bw][:, hs], qTdz[:, p, h % 2, :], Sbf[si][:, p, :], False, True,
                   ['qTd', ('Sbf', si)], [('ps', bw)])
            for p in range(2):
                for h2 in range(2):
                    rw = slice(h2 * 64, h2 * 64 + 64)
                    c0 = p * 256 + h2 * P
                    dve(lambda e, p=p, bv=bv, rw=rw, c0=c0: e.scalar_tensor_tensor(
                        out=Sst[rw, p, :], in0=Sst[rw, p, :], scalar=dec[rw, p:p + 1], in1=psb[bv][rw, c0:c0 + P],
                        op0=ALU.mult, op1=ALU.add), ['Sst', 'dec', ('ps', bv)], ['Sst'])
            sn = 1 - si
            pool(lambda e, sn=sn: e.tensor_copy(Sbf[sn][:, :, :], Sst[:, :, :]), ['Sst'], [('Sbf', sn)])
            sidx[0] = sn
            act(lambda e, bw=bw: e.activation(out=sq_t[:, :], in_=psb[bw][:, :], func=AF.Square),
                [('ps', bw)], ['sq_t'])
            dve(lambda e: e.tensor_reduce(out=st[:, 12:16], in_=sq_t[:, :].rearrange("p (h e) -> p h e", e=P),
                                          axis=AX.X, op=ALU.add), ['sq_t'], [('st', 12)])
            rstd_from(12, 1.0 / P, 4)
            for h in range(4):
                hs = slice(h * P, (h + 1) * P)
                dve(lambda e, h=h, hs=hs, bw=bw, t=t: e.scalar_tensor_tensor(
                    out=og[:, hs], in0=psb[bw][:, hs], scalar=rs[:, 12 + h:13 + h], in1=sg_tm[:, t, hs],
                    op0=ALU.mult, op1=ALU.mult), [('ps', bw), ('rs', 12), ('sg_tm', t)], ['og'])
            yield (0.7, 4.0)
            bt = bank('m')
            for h in range(4):
                hs = slice(h * P, (h + 1) * P)
                tr(psb16[bt][:, hs], og[:, hs], ['og'], [('ps', bt)])
            dve(lambda e, bt=bt, ts=ts: e.tensor_scalar(
                out=mixT[:, 0:4, ts], in0=psb16[bt][:, 0:512].rearrange("p (h i) -> p h i", i=P),
                scalar1=glaw_t[:, 0:1], scalar2=None, op0=ALU.mult), [('ps', bt), 'glaw'], [('mixT', t)])
            yield (0.3, 0.0)
        def norm2(tp):
            yield 'need_gu_done'
            for tt in range(2):
                t = tp * 2 + tt
                yield from norm_T((b % 2) * 4 + t, t, 4 + t, w2b, 'w2b', n2T, 'n2T', nbf2, 'nbf2', 'm')

        yield from proj_tm(mixT, 'mixT', [(c, c) for c in range(8)], SC_WOUT, 'scrB', b, 'm', after_tp=norm2)

    def ffn(b):
        n2_all = [('n2T', t) for t in range(NT)]

        def finish(tp):
            for tt in range(2):
                t = tp * 2 + tt
                sl = (b % 2) * 4 + t
                col = 8 + t
                act(lambda e, sl=sl, col=col: e.activation(out=junk[:, :], in_=xh[sl][:, :], func=AF.Square,
                                                           accum_out=st[:, col:col + 1]), [('xh', sl)], [('st', col)])
                rstd_from(col, 1.0 / D)
                dve(lambda e, sl=sl, col=col: e.scalar_tensor_tensor(
                    out=xh[sl][:, :], in0=xh[sl][:, :], scalar=rs[:, col:col + 1], in1=wfb[:, :],
                    op0=ALU.mult, op1=ALU.mult), [('xh', sl), ('rs', col), 'wfb'], [('xh', sl)])
                r0 = b * TB + t * P
                pdma(out[r0:r0 + P, :], xh[sl][:, :], [('xh', sl)], [('out', b, t)], ('xs', sl))
            if b + 2 < nblk:
                load_x(b + 2, [tp * 2, tp * 2 + 1])

        for pas in range(2):
            for jj in range(11):
                j = pas * 11 + jj
                bks = []
                for gu in range(2):
                    s = ring_next(SC_GU + 2 * j + gu, 'scrC%d' % (j // 3))
                    bk = bank('f')
                    bks.append(bk)
                    for k in range(KD):
                        mm(psb[bk][:, :], ring[:, s, k * P:(k + 1) * P], n2T[:, k, :], k == 0, k == KD - 1,
                           n2_all + [('ring', s)], [('ps', bk)])
                    yield (1.73, 0.0)
                tf = tmpF[jj % 2]
                tk = ('tmpF', jj % 2)
                sigmoid_act(tf[:, :], psb[bks[0]][:, :], tk, ('ps', bks[0]))
                dve(lambda e, bk=bks[0], tf=tf: e.tensor_tensor(out=tf[:, :], in0=tf[:, :], in1=psb[bk][:, :], op=ALU.mult),
                    [tk, ('ps', bks[0])], [tk])
                dve(lambda e, bk=bks[1], tf=tf, jj=jj: e.tensor_tensor(
                    out=gT[:, jj, :], in0=tf[:, :], in1=psb[bk][:, :], op=ALU.mult),
                    [tk, ('ps', bks[1])], [('gT', t_) for t_ in range(NT)])
            if pas == 1:
                yield 'gu_done'
            yield (0.0, 3.0)
            yield from proj_tm(gT, 'gT', [(jj, pas * 11 + jj) for jj in range(11)], SC_D, (lambda j_: 'scrD%d' % (j_ // 6)), b, 'f',
                               after_tp=(finish if pas == 1 else None))

    def run_alone(g):
        for _ in g:
            pass

    def interleave(ga, gb):
        tnow = 0.0
        rdy = [0.0, 0.0]
        done = [False, False]
        gens = [ga, gb]
        gu_done = False
        blocked = False
        while not (done[0] and done[1]):
            cands = [i for i in (0, 1) if not done[i] and not (i == 1 and blocked and not gu_done)]
            i = min(cands, key=lambda i_: (max(rdy[i_], tnow), -i_))
            try:
                y = next(gens[i])
            except StopIteration:
                done[i] = True
                if i == 0:
                    gu_done = True
                continue
            if y == 'gu_done':
                gu_done = True
                continue
            if y == 'need_gu_done':
                blocked = True
                continue
            pe_c, lat = y
            lat = lat * LATSCALE
            tnow = max(tnow, rdy[i]) + pe_c
            rdy[i] = tnow + lat

    run_alone(mixer(0))
    for b in range(nblk):
        if b + 1 < nblk:
            interleave(ffn(b), mixer(b + 1))
        else:
            run_alone(ffn(b))
    S.add('pool', None, [('out', b, t) for b in range(nblk) for t in range(NT)], [])

    with nc.allow_low_precision(reason="bf16 matmul operands, fp32 accumulate"):
        with nc.allow_non_contiguous_dma(reason="weight re-layout"):
            S.emit(nc)
    return nc


_CACHE = {}


def _consts():
    i = np.arange(P)
    U = (i[:, None] <= i[None, :]).astype(np.float32)
    SUm = (i[:, None] > i[None, :]).astype(np.float32)
    bf = ml_dtypes.bfloat16
    return dict(c_ident=np.eye(P, dtype=np.float32).astype(bf),
                c_u4=np.tile(U, (1, 4)).astype(bf),
                c_su=SUm.astype(bf))


def prep_shared(inp):
    f = lambda a: np.ascontiguousarray(np.asarray(a, dtype=np.float32))
    d = dict(
        w_in=f(inp["w_in"][0]), w_out=f(inp["w_out"][0]), w_g=f(inp["w_ffn_gate"][0]),
        w_u=f(inp["w_ffn_up"][0]), w_d=f(inp["w_ffn_down"][0]),
        n1b=f(np.broadcast_to(np.asarray(inp["norm1_w"][0]), (P, D))),
        n2b=f(np.broadcast_to(np.asarray(inp["norm2_w"][0]), (P, D))),
        nfb=f(np.broadcast_to(np.asarray(inp["final_norm_w"]), (P, D))),
        wgu=f(np.concatenate([np.asarray(inp["w_gate_up"][0]), np.asarray(inp["b_gate_up"][0])[None, :]], axis=0)),
        glaw=f(np.asarray(inp["gla_norm_w"][0]).reshape(P, 1)),
        lnw=f(np.asarray(inp["sg_ln_w"][0]).T), lnb=f(np.asarray(inp["sg_ln_b"][0]).T),
        wsT=f(np.asarray(inp["sg_w_s"][0]).transpose(2, 0, 1).reshape(P, 512)),
        bsb=f(np.broadcast_to(np.asarray(inp["sg_b_s"][0]).reshape(1, 512), (P, 512))),
    )
    d.update(_consts())
    return d


def kernel(**inputs):
    x = np.asarray(inputs["x"], dtype=np.float32)
    B, T, _ = x.shape
    n_tok = (B // NCORES) * T
    key = (n_tok, T)
    if key not in _CACHE:
        _CACHE[key] = build(n_tok, T)
    nc = _CACHE[key]
    shared = prep_shared(inputs)
    xs = x.reshape(NCORES, n_tok, D)
    in_maps = []
    for c in range(NCORES):
        m = dict(shared)
        m["x"] = np.ascontiguousarray(xs[c])
        in_maps.append(m)
    res = run_bass_kernel_spmd(nc, in_maps, core_ids=list(range(NCORES)))
    outs = [np.asarray(r["out"], dtype=np.float32) for r in res.results]
    return np.stack(outs, axis=0).reshape(B, T, D)
```
